# Optimizing a Trainium2 kernel written in Bass

```python
import jax, jax.numpy as jnp
from jax import lax
import numpy as np

D_MODEL = 1024
BATCH = 4
SEQ = 8192
DEPTH = 2

CHUNK = 64
Q_BLOCK = 128
HEAD_DIM = 64
D_MIX = D_MODEL
A_HEADS = 6
A_LEFT_CHUNKS = 8
A_REL_MAX = 128
A_NUM_REL = CHUNK + A_REL_MAX
B_HEADS = 5
IDX_HEADS = 8
IDX_DIM = 32
TOPK_MAX = 256
C_HEADS = 5
C_Q_RANK = 384
C_KV_RANK = 256
C_NOPE = 64
C_ROPE = 32
C_V = 64
D_FF = 2816
ROPE_THETA = 10000.0
EPS = 1e-6
NEG = -1e30

A_W = A_HEADS * HEAD_DIM
B_W = B_HEADS * HEAD_DIM
C_W = C_HEADS * C_V
IN_SIZES = (A_W, A_W, A_W, B_W, B_W, B_W, IDX_HEADS * IDX_DIM, IDX_DIM, IDX_HEADS,
            C_Q_RANK, C_KV_RANK, C_ROPE)
D_IN = 3 * A_W + 3 * B_W + IDX_HEADS * IDX_DIM + IDX_DIM + IDX_HEADS + C_Q_RANK + C_KV_RANK + C_ROPE

kernel_name = "hybrid_streaming_chunkattn_dsa_mla_macaron"


def rmsnorm(x, g):
    x32 = x.astype(jnp.float32)
    y = x32 * lax.rsqrt(jnp.mean(x32 * x32, axis=-1, keepdims=True) + EPS)
    return (y * g.astype(jnp.float32)).astype(x.dtype)


def rope(x, pos):
    d = x.shape[-1]
    inv = ROPE_THETA ** (-jnp.arange(0, d, 2, dtype=jnp.float32) / d)
    ang = pos.astype(jnp.float32)[:, None] * inv[None, :]
    cos = jnp.cos(ang)[:, None, :]
    sin = jnp.sin(ang)[:, None, :]
    x32 = x.astype(jnp.float32)
    x1, x2 = x32[..., : d // 2], x32[..., d // 2:]
    return jnp.concatenate([x1 * cos - x2 * sin, x1 * sin + x2 * cos], axis=-1).astype(x.dtype)


def swiglu(x, w_gate, w_up, w_down):
    return (jax.nn.silu(x @ w_gate) * (x @ w_up)) @ w_down


def sweep_query_blocks(fn, bsz, seq):
    out = lax.map(fn, jnp.arange(seq // Q_BLOCK) * Q_BLOCK)
    return jnp.moveaxis(out, 0, 1).reshape(bsz, seq, -1)


def chunk_relpos_attention(q, k, v, rel_bias):
    bsz, seq, h, d = q.shape
    nc = seq // CHUNK
    nb = A_LEFT_CHUNKS + 1
    pad = A_LEFT_CHUNKS * CHUNK
    qc = q.reshape(bsz, nc, CHUNK, h, d)
    kp = jnp.pad(k, ((0, 0), (pad, 0), (0, 0), (0, 0))).reshape(bsz, nc + A_LEFT_CHUNKS, CHUNK, h, d)
    vp = jnp.pad(v, ((0, 0), (pad, 0), (0, 0), (0, 0))).reshape(bsz, nc + A_LEFT_CHUNKS, CHUNK, h, d)
    kb = jnp.stack([kp[:, j:j + nc] for j in range(nb)], axis=2).reshape(bsz, nc, nb * CHUNK, h, d)
    vb = jnp.stack([vp[:, j:j + nc] for j in range(nb)], axis=2).reshape(bsz, nc, nb * CHUNK, h, d)
    s = jnp.einsum('bcqhd,bckhd->bhcqk', qc, kb).astype(jnp.float32) * (d ** -0.5)
    qi = jnp.arange(CHUNK)[:, None]
    ki = jnp.arange(nb * CHUNK)[None, :]
    rel = pad + qi - ki
    ridx = jnp.clip(rel, -(CHUNK - 1), A_REL_MAX) + (CHUNK - 1)
    bias = rel_bias.astype(jnp.float32)[:, ridx]
    valid = (jnp.arange(nc)[:, None] + ki // CHUNK - A_LEFT_CHUNKS) >= 0
    s = jnp.where(valid[None, None, :, None, :], s + bias[None, :, None], NEG)
    p = jax.nn.softmax(s, axis=-1).astype(v.dtype)
    o = jnp.einsum('bhcqk,bckhd->bcqhd', p, vb)
    return o.reshape(bsz, seq, h * d)


def dsa_attention(q, k, v, q_idx, k_idx, w_idx):
    bsz, seq, h, d = q.shape
    topk = min(TOPK_MAX, seq // 4)
    kchunk = jnp.arange(seq) // CHUNK
    gather = jax.vmap(lambda kk, ii: kk[ii])

    def block(q0):
        tc = (q0 + jnp.arange(Q_BLOCK)) // CHUNK
        qi = lax.dynamic_slice_in_dim(q_idx, q0, Q_BLOCK, axis=1)
        wi = lax.dynamic_slice_in_dim(w_idx, q0, Q_BLOCK, axis=1).astype(jnp.float32) * (IDX_HEADS ** -0.5)
        qb = lax.dynamic_slice_in_dim(q, q0, Q_BLOCK, axis=1)
        dots = jnp.einsum('bqhd,bsd->bqhs', qi, k_idx).astype(jnp.float32) * (IDX_DIM ** -0.5)
        score = jnp.einsum('bqh,bqhs->bqs', wi, jax.nn.relu(dots))
        adm = kchunk[None, :] <= tc[:, None]
        score = jnp.where(adm[None], score, NEG)
        _, idx = lax.top_k(score, topk)
        ksel = gather(k, idx)
        vsel = gather(v, idx)
        valid = (idx // CHUNK) <= tc[None, :, None]
        s = jnp.einsum('bqhd,bqkhd->bhqk', qb, ksel).astype(jnp.float32) * (d ** -0.5)
        s = jnp.where(valid[:, None], s, NEG)
        p = jax.nn.softmax(s, axis=-1).astype(v.dtype)
        return jnp.einsum('bhqk,bqkhd->bqhd', p, vsel)

    return sweep_query_blocks(block, bsz, seq)


def mla_attention(c_q, c_kv, k_rope, q_norm, kv_norm, w_uq, w_ukv, pos):
    bsz, seq, _ = c_q.shape
    q = (rmsnorm(c_q, q_norm) @ w_uq).reshape(bsz, seq, C_HEADS, C_NOPE + C_ROPE)
    qn, qr = q[..., :C_NOPE], rope(q[..., C_NOPE:], pos)
    kv = (rmsnorm(c_kv, kv_norm) @ w_ukv).reshape(bsz, seq, C_HEADS, C_NOPE + C_V)
    kn, v = kv[..., :C_NOPE], kv[..., C_NOPE:]
    kr = rope(k_rope[:, :, None, :], pos)[:, :, 0, :]
    kchunk = pos // CHUNK
    scale = (C_NOPE + C_ROPE) ** -0.5

    def block(q0):
        tc = (q0 + jnp.arange(Q_BLOCK)) // CHUNK
        qnb = lax.dynamic_slice_in_dim(qn, q0, Q_BLOCK, axis=1)
        qrb = lax.dynamic_slice_in_dim(qr, q0, Q_BLOCK, axis=1)
        s = (jnp.einsum('bqhd,bshd->bhqs', qnb, kn) + jnp.einsum('bqhr,bsr->bhqs', qrb, kr)).astype(jnp.float32) * scale
        s = jnp.where((kchunk[None, :] <= tc[:, None])[None, None], s, NEG)
        p = jax.nn.softmax(s, axis=-1).astype(v.dtype)
        return jnp.einsum('bhqs,bshd->bqhd', p, v)

    return sweep_query_blocks(block, bsz, seq)


def split_columns(h):
    parts, off = [], 0
    for size in IN_SIZES:
        parts.append(h[..., off:off + size])
        off += size
    return parts


def hybrid_layer(x, pos, ffn1_norm, ffn1_w_gate, ffn1_w_up, ffn1_w_down, mix_norm, w_in, a_rel_bias,
                 c_q_norm, c_kv_norm, c_w_uq, c_w_ukv, w_out, ffn2_norm, ffn2_w_gate, ffn2_w_up, ffn2_w_down):
    bsz, seq, _ = x.shape
    x = x + 0.5 * swiglu(rmsnorm(x, ffn1_norm), ffn1_w_gate, ffn1_w_up, ffn1_w_down)
    h = rmsnorm(x, mix_norm) @ w_in
    (a_q, a_k, a_v, b_q, b_k, b_v, i_q, i_k, i_w, c_q, c_kv, c_kr) = split_columns(h)
    hd4 = lambda t, nh: t.reshape(bsz, seq, nh, -1)
    o_a = chunk_relpos_attention(hd4(a_q, A_HEADS), hd4(a_k, A_HEADS), hd4(a_v, A_HEADS), a_rel_bias)
    o_b = dsa_attention(rope(hd4(b_q, B_HEADS), pos), rope(hd4(b_k, B_HEADS), pos), hd4(b_v, B_HEADS),
                        rope(hd4(i_q, IDX_HEADS), pos), rope(i_k[:, :, None, :], pos)[:, :, 0, :], i_w)
    o_c = mla_attention(c_q, c_kv, c_kr, c_q_norm, c_kv_norm, c_w_uq, c_w_ukv, pos)
    x = x + jnp.concatenate([o_a, o_b, o_c], axis=-1) @ w_out
    x = x + 0.5 * swiglu(rmsnorm(x, ffn2_norm), ffn2_w_gate, ffn2_w_up, ffn2_w_down)
    return x


def setup_inputs(seed: int = 0) -> dict:
    key = jax.random.key(seed)
    ks = jax.random.split(key, 20)
    f32 = jnp.float32

    def nrm(k, shape, scale):
        return jax.random.normal(k, shape, f32) * scale

    def gain(k, n):
        return 1.0 + 0.02 * jax.random.normal(k, (DEPTH, n), f32)

    return {
        "x": jax.random.normal(ks[0], (BATCH, SEQ, D_MODEL), f32),
        "ffn1_norm": gain(ks[1], D_MODEL),
        "ffn1_w_gate": nrm(ks[2], (DEPTH, D_MODEL, D_FF), D_MODEL ** -0.5),
        "ffn1_w_up": nrm(ks[3], (DEPTH, D_MODEL, D_FF), D_MODEL ** -0.5),
        "ffn1_w_down": nrm(ks[4], (DEPTH, D_FF, D_MODEL), D_FF ** -0.5),
        "mix_norm": gain(ks[5], D_MODEL),
        "w_in": nrm(ks[6], (DEPTH, D_MODEL, D_IN), D_MODEL ** -0.5),
        "a_rel_bias": nrm(ks[7], (DEPTH, A_HEADS, A_NUM_REL), 0.1),
        "c_q_norm": gain(ks[8], C_Q_RANK),
        "c_kv_norm": gain(ks[9], C_KV_RANK),
        "c_w_uq": nrm(ks[10], (DEPTH, C_Q_RANK, C_HEADS * (C_NOPE + C_ROPE)), C_Q_RANK ** -0.5),
        "c_w_ukv": nrm(ks[11], (DEPTH, C_KV_RANK, C_HEADS * (C_NOPE + C_V)), C_KV_RANK ** -0.5),
        "w_out": nrm(ks[12], (DEPTH, D_MIX, D_MODEL), D_MIX ** -0.5),
        "ffn2_norm": gain(ks[13], D_MODEL),
        "ffn2_w_gate": nrm(ks[14], (DEPTH, D_MODEL, D_FF), D_MODEL ** -0.5),
        "ffn2_w_up": nrm(ks[15], (DEPTH, D_MODEL, D_FF), D_MODEL ** -0.5),
        "ffn2_w_down": nrm(ks[16], (DEPTH, D_FF, D_MODEL), D_FF ** -0.5),
        "final_norm": 1.0 + 0.02 * jax.random.normal(ks[17], (D_MODEL,), f32),
    }


def reference(x, ffn1_norm, ffn1_w_gate, ffn1_w_up, ffn1_w_down, mix_norm, w_in, a_rel_bias,
              c_q_norm, c_kv_norm, c_w_uq, c_w_ukv, w_out, ffn2_norm, ffn2_w_gate, ffn2_w_up,
              ffn2_w_down, final_norm):
    pos = jnp.arange(x.shape[1], dtype=jnp.int32)
    for l in range(DEPTH):
        x = hybrid_layer(x, pos, ffn1_norm[l], ffn1_w_gate[l], ffn1_w_up[l], ffn1_w_down[l], mix_norm[l],
                         w_in[l], a_rel_bias[l], c_q_norm[l], c_kv_norm[l], c_w_uq[l], c_w_ukv[l], w_out[l],
                         ffn2_norm[l], ffn2_w_gate[l], ffn2_w_up[l], ffn2_w_down[l])
    return rmsnorm(x, final_norm)
```

```python
import numpy as np
import ml_dtypes
from contextlib import ExitStack
import concourse.bass as bass
import concourse.mybir as mybir
from concourse.bass_utils import run_bass_kernel_spmd

F32 = mybir.dt.float32
BF16 = mybir.dt.bfloat16
AF = mybir.ActivationFunctionType
ALU = mybir.AluOpType
AX = mybir.AxisListType

D = 1024
DFF = 2816
NKC = 8
NFC = 22
EPS = 1e-6
NEGM = -30000.0
ROPE_THETA = 10000.0

ENGS = ("pe", "act", "dve", "pool", "sp")
ROT = 16000


class Op:
    __slots__ = ("eng", "fn", "deps", "dkey", "needed", "sig", "idx")

    def __init__(self, eng, fn, deps, dkey):
        self.eng = eng
        self.fn = fn
        self.deps = deps
        self.dkey = dkey
        self.needed = False
        self.sig = None


class Buf:
    __slots__ = ("w", "r")

    def __init__(self):
        self.w = []
        self.r = []


class Sched:
    def __init__(self):
        self.q = {e: [] for e in ENGS}
        self.dkeys = {}

    def add(self, eng, fn, reads=(), writes=(), deps=(), dkey=None, acc=False):
        dl = [d for d in deps if d is not None]
        for b in reads:
            dl.extend(b.w)
        if not acc:
            for b in writes:
                dl.extend(b.w)
                dl.extend(b.r)
        op = Op(eng, fn, dl, dkey)
        for d in dl:
            d.needed = True
        for b in reads:
            b.r.append(op)
        for b in writes:
            b.w = [op]
            b.r = []
        if dkey is not None:
            self.dkeys.setdefault(dkey, 0)
        self.q[eng].append(op)
        return op

    def emit(self, nc, final_waits=()):
        for d in final_waits:
            d.needed = True
        with ExitStack() as es:
            nsig = {e: sum(1 for o in self.q[e] if o.needed and o.dkey is None) for e in ENGS}
            esems = {e: [es.enter_context(nc.semaphore(f"s_{e}{k}")) for k in range((nsig[e] + ROT - 1) // ROT)]
                     for e in ENGS}
            dsems = {k: es.enter_context(nc.semaphore(f"d_{i}")) for i, k in enumerate(self.dkeys)}
            dcnt = {k: 0 for k in self.dkeys}
            for e in ENGS:
                c = 0
                for o in self.q[e]:
                    if o.dkey is not None:
                        dcnt[o.dkey] += 16
                        o.sig = (dsems[o.dkey], dcnt[o.dkey])
                    elif o.needed:
                        o.sig = (esems[e][c // ROT], c % ROT + 1)
                        c += 1
            block = es.enter_context(nc.Block())
            engmap = {"pe": "tensor", "act": "scalar", "dve": "vector", "pool": "gpsimd", "sp": "sync"}

            def run(ename, eng, last=False):
                waited = {}
                for o in self.q[ename]:
                    need = {}
                    for d in o.deps:
                        s, v = d.sig
                        if need.get(id(s), (None, 0))[1] < v:
                            need[id(s)] = (s, v)
                    for sid, (s, v) in need.items():
                        if waited.get(sid, 0) < v:
                            eng.wait_ge(s, v)
                            waited[sid] = v
                    ins = o.fn(eng)
                    if o.dkey is not None:
                        ins.then_inc(o.sig[0], 16)
                    elif o.needed:
                        ins.then_inc(o.sig[0], 1)
                if last:
                    fw = {}
                    for d in final_waits:
                        s, v = d.sig
                        if fw.get(id(s), (None, 0))[1] < v:
                            fw[id(s)] = (s, v)
                    for s, v in fw.values():
                        eng.wait_ge(s, v)

            @block.tensor
            def _(e):
                run("pe", e)

            @block.scalar
            def _(e):
                run("act", e)

            @block.vector
            def _(e):
                run("dve", e)

            @block.gpsimd
            def _(e):
                run("pool", e)

            @block.sync
            def _(e):
                run("sp", e, last=True)


class Ctx:
    def __init__(self):
        self.nc = bass.Bass("TRN2", target_bir_lowering=False)
        self.s = Sched()
        self.es = ExitStack()
        self.n = 0
        self.banks = [self.es.enter_context(self.nc.psum_tensor(f"ps{i}", [128, 512], F32)) for i in range(8)]
        self.bankb = [Buf() for _ in range(8)]
        self.bank_rr = 0
        self.outs = []

    def sb(self, shape, dt, name=None):
        self.n += 1
        return self.es.enter_context(self.nc.sbuf_tensor(f"sb{self.n}_" + (name or "t"), list(shape), dt))

    def dram(self, name, shape, dt, kind):
        return self.nc.dram_tensor(name, list(shape), dt, kind=kind).ap()

    def bank(self, allowed=None):
        allowed = allowed or range(8)
        allowed = list(allowed)
        i = allowed[self.bank_rr % len(allowed)]
        self.bank_rr += 1
        return self.banks[i], self.bankb[i]

    def finish(self):
        self.s.emit(self.nc, final_waits=self.outs)
        self.es.close()
        return self.nc


class Ring:
    def __init__(self, cx, n, shape, dt, name):
        self.t = [cx.sb(shape, dt, f"{name}{i}") for i in range(n)]
        self.b = [Buf() for _ in range(n)]
        self.i = 0
        self.name = name

    def next(self):
        k = self.i % len(self.t)
        self.i += 1
        return self.t[k], self.b[k], f"{self.name}{k}"


A_W, B_W = 384, 320
OFF = {}
_o = 0
for _n, _s in (("a_q", 384), ("a_k", 384), ("a_v", 384), ("b_q", 320), ("b_k", 320), ("b_v", 320),
               ("i_q", 256), ("i_k", 32), ("i_w", 8), ("c_q", 384), ("c_kv", 256), ("c_kr", 32)):
    OFF[_n] = _o
    _o += _s
D_IN = _o


def _swap_idx(base, nheads, d):
    idx = []
    for h in range(nheads):
        for j in range(d):
            idx.append(base + h * d + (j + d // 2) % d)
    return idx


def win_fm_columns():
    ch = []

    def pad(cols):
        cols = list(cols)
        return cols + [cols[-1]] * (128 - len(cols))

    for nm in ("a_q", "a_k"):
        for c in range(3):
            ch.append((f"{nm}{c}", pad(range(OFF[nm] + 128 * c, OFF[nm] + 128 * (c + 1)))))
    for nm in ("b_q", "b_k"):
        base = OFF[nm]
        sw = _swap_idx(base, 5, 64)
        for c in range(3):
            lo, hi = 128 * c, min(128 * (c + 1), 320)
            ch.append((f"{nm}{c}", pad(range(base + lo, base + hi))))
            ch.append((f"{nm}{c}s", pad(sw[lo:hi])))
    base = OFF["i_q"]
    sw = _swap_idx(base, 8, 32)
    for c, (h0, h1) in enumerate(((0, 3), (3, 6), (6, 8))):
        ch.append((f"i_q{c}", pad(range(base + 32 * h0, base + 32 * h1))))
        ch.append((f"i_q{c}s", pad(sw[32 * h0:32 * h1])))
    base = OFF["i_k"]
    ch.append(("i_k", list(range(base, base + 32)) * 4))
    ch.append(("i_ks", _swap_idx(base, 1, 32) * 4))
    for c in range(3):
        ch.append((f"c_q{c}", pad(range(OFF["c_q"] + 128 * c, OFF["c_q"] + 128 * (c + 1)))))
    for c in range(2):
        ch.append((f"c_kv{c}", pad(range(OFF["c_kv"] + 128 * c, OFF["c_kv"] + 128 * (c + 1)))))
    base = OFF["c_kr"]
    kr = list(range(base, base + 32))
    ch.append(("c_kr", pad(kr + kr + kr)))
    krs = _swap_idx(base, 1, 32)
    ch.append(("c_krs", pad(krs + krs + krs)))
    return ch


WIN_CH = win_fm_columns()
WIN_IDX = {n: i for i, (n, _) in enumerate(WIN_CH)}
NWC = len(WIN_CH)
WIN_TM_COLS = list(range(OFF["a_v"], OFF["a_v"] + 384)) + list(range(OFF["b_v"], OFF["b_v"] + 320)) + \
    list(range(OFF["i_w"], OFF["i_w"] + 8))
NTM = len(WIN_TM_COLS)


def uq_columns():
    ch = []
    for h in range(5):
        b = h * 96
        ch.append(list(range(b, b + 96)))
    for h in range(5):
        b = h * 96
        ch.append(list(range(b, b + 64)) + [b + 64 + (j + 16) % 32 for j in range(32)])
    return ch


def chunked(w, cw=128):
    K, N = w.shape
    return np.ascontiguousarray(w.reshape(K // 128, 128, N // cw, cw).transpose(2, 1, 0, 3))


def rowfmt(w):
    K, N = w.shape
    return np.ascontiguousarray(w.reshape(K // 128, 128, N).transpose(1, 0, 2))


def tile_of(r, j):
    i, sl = divmod(j, 2)
    if r == 0:
        return 4 * i + (0 if sl == 0 else 3)
    return 4 * i + (1 if sl == 0 else 2)


def rope_tables(pos):
    pos = np.asarray(pos, np.float32)
    out = []
    for d, rep in ((64, 2), (32, 4)):
        inv = (ROPE_THETA ** (-np.arange(0, d, 2, dtype=np.float32) / d)).astype(np.float32)
        ang = pos[None, :] * inv[:, None]
        cos = np.cos(ang).astype(np.float32)
        sin = np.sin(ang).astype(np.float32)
        c = np.concatenate([cos, cos], 0)
        s = np.concatenate([-sin, sin], 0)
        out.append(np.ascontiguousarray(np.tile(c, (rep, 1))))
        out.append(np.ascontiguousarray(np.tile(s, (rep, 1))))
    return out


def I(name, *args, **kw):
    return lambda e: getattr(e, name)(*args, **kw)


def dma(cx, q, out, in_, reads=(), writes=(), deps=(), dkey=None):
    return cx.s.add(q, I("dma_start", out=out, in_=in_), reads=reads, writes=writes, deps=deps, dkey=dkey)


def barrier(cx, extra=()):
    lasts = [cx.s.q[e][-1] for e in ENGS if cx.s.q[e]] + list(extra)
    scr = cx.sb([128, 4], F32)
    for i, e in enumerate(("act", "dve", "pool")):
        cx.s.add(e, (lambda e_, i=i: e_.memset(scr[:, i:i + 1], 0.0)) if e != "act" else
                 (lambda e_, i=i: e_.mul(out=scr[:, i:i + 1], in_=scr[:, i:i + 1], mul=0.0)), deps=lasts)
    cx.s.add("sp", lambda e_: e_.dma_start(out=scr[0:1, 3:4], in_=scr[0:1, 2:3]),
             deps=lasts + [cx.s.q[e][-1] for e in ("act", "dve", "pool")], dkey=f"bar{cx.n}")
    cx.n += 1


class Caster:
    def __init__(self, cx, maxf):
        self.cx = cx
        self.r32 = Ring(cx, 3, [128, maxf], F32, "c32_")
        self.r16 = Ring(cx, 3, [128, maxf], BF16, "c16_")
        self.k = 0
        self.last = []

    def cast(self, src, dst, f):
        cx = self.cx
        t32, b32, k32 = self.r32.next()
        t16, b16, k16 = self.r16.next()
        dma(cx, "sp", t32[:, 0:f], src, writes=[b32], dkey="ld" + k32)
        eng = ("dve", "act", "pool")[self.k % 3]
        self.k += 1
        if eng == "act":
            cx.s.add("act", I("copy", out=t16[:, 0:f], in_=t32[:, 0:f]), reads=[b32], writes=[b16])
        else:
            cx.s.add(eng, I("tensor_copy", out=t16[:, 0:f], in_=t32[:, 0:f]), reads=[b32], writes=[b16])
        op = dma(cx, "sp", dst, t16[:, 0:f], reads=[b16], dkey="st" + k16)
        self.last.append(op)
        return op


def emit_T(cx, Town, TT, pre, post, final, W):
    s = cx.s
    nc = cx.nc
    NT = Town // TT
    NH = TT // 512
    NS = TT // 128
    ones32 = cx.sb([128, 128], F32, "ones32")
    ones_b = Buf()
    s.add("dve", I("memset", ones32[:], 1.0), writes=[ones_b])
    gains = {}
    gb = Buf()
    for nm, n in (("g_ffn1", 8), ("g_mix", 8), ("g_ffn2", 8), ("g_final", 8), ("g_cq", 3), ("g_ckv", 2)):
        if nm in W:
            t = cx.sb([128, n], F32, "sb_" + nm)
            dma(cx, "sp", t[:], W[nm], writes=[gb], dkey="gains")
            gains[nm] = t
    gb.w = gb.w[-1:]

    xT = cx.sb([128, NKC, TT], F32, "xT")
    xT_b = [Buf() for _ in range(NKC)]
    xn = cx.sb([128, NKC, TT], BF16, "xn")
    xn_b = [Buf() for _ in range(NKC)]
    hT = cx.sb([128, NFC, TT], BF16, "hT")
    hT_b = [Buf() for _ in range(NFC)]
    sq_r = Ring(cx, 2, [128, 512], F32, "sq")
    rstd = cx.sb([128, TT], F32, "rstd")
    rstd_b = Buf()
    lnv = cx.sb([128, TT], F32, "lnv")
    lnv_b = Buf()
    gs_r = Ring(cx, 2, [128, 512], F32, "gs")
    wg_r = Ring(cx, 3, [128, NKC, 128], BF16, "wg")
    wu_r = Ring(cx, 3, [128, NKC, 128], BF16, "wu")
    wd_r = Ring(cx, 2, [128, NFC, 128], BF16, "wd")
    w8_r = Ring(cx, 4, [128, NKC, 128], BF16, "w8")

    def rmsnorm(src, src_b, nch, gain, dst, dst_b, width):
        for h in range(NH):
            cs = slice(h * 512, (h + 1) * 512)
            bk, bb = cx.bank()
            for c in range(nch):
                sq, sqb, _ = sq_r.next()
                s.add("act", I("activation", out=sq[:], in_=src[:, c, cs], func=AF.Square),
                      reads=[src_b[c]], writes=[sqb])
                s.add("pe", I("matmul", bk[:], lhsT=ones32[:], rhs=sq[:],
                                                                 start=(c == 0), stop=(c == nch - 1)),
                      reads=[sqb, ones_b], writes=[bb], acc=(c != 0))
            s.add("act", I("activation", out=lnv[:, cs], in_=bk[:], func=AF.Ln,
                                                       scale=1.0 / width, bias=eps_t[:]),
                  reads=[bb, eps_b], writes=[lnv_b] if h == 0 else [], deps=lnv_b.w if h else ())
            s.add("act", I("activation", out=rstd[:, cs], in_=lnv[:, cs], func=AF.Exp, scale=-0.5),
                  reads=[lnv_b], writes=[rstd_b] if h == 0 else [], deps=(rstd_b.w + rstd_b.r) if h else ())
        for c in range(nch):
            s.add("dve", I("scalar_tensor_tensor", out=dst[:, c, :], in0=src[:, c, :],
                                                               scalar=gain[:, c:c + 1], in1=rstd[:],
                                                               op0=ALU.mult, op1=ALU.mult),
                  reads=[src_b[c], rstd_b, gb], writes=[dst_b[c]])

    eps_t = cx.sb([128, 1], F32, "eps")
    eps_b = Buf()
    s.add("dve", I("memset", eps_t[:], EPS), writes=[eps_b])

    def ffn(wg, wu, wd, gain):
        rmsnorm(xT, xT_b, NKC, gain, xn, xn_b, D)
        for j in range(NFC):
            g_t, g_b, gk = wg_r.next()
            u_t, u_b, uk = wu_r.next()
            dma(cx, "sp", g_t[:], wg[j], writes=[g_b], dkey=gk)
            dma(cx, "sp", u_t[:], wu[j], writes=[u_b], dkey=uk)
            for h in range(NH):
                cs = slice(h * 512, (h + 1) * 512)
                bg, bgb = cx.bank()
                bu, bub = cx.bank()
                for kc in range(NKC):
                    s.add("pe", I("matmul", bg[:], lhsT=g_t[:, kc, :], rhs=xn[:, kc, cs],
                                                                          start=(kc == 0), stop=(kc == NKC - 1)),
                          reads=[g_b, xn_b[kc]], writes=[bgb], acc=(kc != 0))
                for kc in range(NKC):
                    s.add("pe", I("matmul", bu[:], lhsT=u_t[:, kc, :], rhs=xn[:, kc, cs],
                                                                          start=(kc == 0), stop=(kc == NKC - 1)),
                          reads=[u_b, xn_b[kc]], writes=[bub], acc=(kc != 0))
                gs, gsb, _ = gs_r.next()
                s.add("act", I("activation", out=gs[:], in_=bg[:], func=AF.Silu),
                      reads=[bgb], writes=[gsb])
                s.add("dve", I("tensor_tensor", out=hT[:, j, cs], in0=bu[:], in1=gs[:],
                                                                          op=ALU.mult),
                      reads=[bub, gsb], writes=[hT_b[j]] if h == 0 else [], deps=hT_b[j].w if h else ())
        for m in range(NKC):
            d_t, d_b, dk = wd_r.next()
            dma(cx, "sp", d_t[:], wd[m], writes=[d_b], dkey=dk)
            for h in range(NH):
                cs = slice(h * 512, (h + 1) * 512)
                bd, bdb = cx.bank()
                for j in range(NFC):
                    s.add("pe", I("matmul", bd[:], lhsT=d_t[:, j, :], rhs=hT[:, j, cs],
                                                                        start=(j == 0), stop=(j == NFC - 1)),
                          reads=[d_b, hT_b[j]], writes=[bdb], acc=(j != 0))
                s.add("dve", I("scalar_tensor_tensor", out=xT[:, m, cs], in0=bd[:], scalar=0.5,
                                                                          in1=xT[:, m, cs], op0=ALU.mult, op1=ALU.add),
                      reads=[bdb], writes=[xT_b[m]] if h == 0 else [],
                      deps=(xT_b[m].w if h else ()))

    if post:
        wtm = cx.sb([128, NKC, NTM], BF16, "wtm")
        wtm_b = Buf()
        dma(cx, "sp", wtm[:], W["win_tm"], writes=[wtm_b], dkey="wtm")
        wuq = cx.sb([128, 10, 3, 96], BF16, "wuq")
        wuq_b = Buf()
        dma(cx, "sp", wuq[:], W["wuq"], writes=[wuq_b], dkey="wuq")
        wkk = cx.sb([128, 5, 2, 64], BF16, "wkk")
        wkk_b = Buf()
        dma(cx, "sp", wkk[:], W["wukv_k"], writes=[wkk_b], dkey="wkk")
        wkv = cx.sb([128, 2, 320], BF16, "wkv")
        wkv_b = Buf()
        dma(cx, "sp", wkv[:], W["wukv_v"], writes=[wkv_b], dkey="wkv")
        ropet = [cx.sb([128, TT], F32, f"rope{i}") for i in range(4)]
        rope_b = Buf()
        cq32 = cx.sb([128, 3, TT], F32, "cq32")
        cq32_b = [Buf() for _ in range(3)]
        ckv32 = cx.sb([128, 2, TT], F32, "ckv32")
        ckv32_b = [Buf() for _ in range(2)]
        cqn = cx.sb([128, 3, TT], BF16, "cqn")
        cqn_b = [Buf() for _ in range(3)]
        ckvn = cx.sb([128, 2, TT], BF16, "ckvn")
        ckvn_b = [Buf() for _ in range(2)]
        st_r = Ring(cx, 4, [128, TT], BF16, "stg")
        t1_r = Ring(cx, 2, [128, 512], F32, "t1_")
        t2_r = Ring(cx, 2, [128, 512], F32, "t2_")
        av_r = Ring(cx, 2, [128, 6, 65], BF16, "avs")
        bv_r = Ring(cx, 2, [128, 5, 65], BF16, "bvs")
        cv_r = Ring(cx, 2, [128, 5, 65], BF16, "cvs")
        iw_r = Ring(cx, 2, [128, 8], F32, "iws")
        for r_ in (av_r, bv_r, cv_r):
            for t_, b_ in zip(r_.t, r_.b):
                s.add("pool", I("memset", t_[:], 1.0), writes=[b_])
    if pre:
        oTs = cx.sb([128, NKC, TT], BF16, "oTs")
        oTs_b = Buf()
    if final:
        fo = cx.sb([128, NKC, TT], F32, "fo")
        fo_b = [Buf() for _ in range(NKC)]

    def rope_evac(bkA, bbA, bkB, bbB, ci, si, dst, dst_rd_wr, prt, cs):
        t1, t1b, _ = t1_r.next()
        t2, t2b, _ = t2_r.next()
        s.add("dve", I("tensor_tensor", out=t1[prt, :], in0=bkA[prt, :], in1=ropet[ci][prt, cs], op=ALU.mult),
              reads=[bbA, rope_b], writes=[t1b])
        s.add("dve", I("tensor_tensor", out=t2[prt, :], in0=bkB[prt, :], in1=ropet[si][prt, cs], op=ALU.mult),
              reads=[bbB, rope_b], writes=[t2b])
        return s.add("pool", I("tensor_tensor", out=dst[prt, cs], in0=t1[prt, :], in1=t2[prt, :], op=ALU.add),
                     reads=[t1b, t2b], **dst_rd_wr)

    for tt in range(NT):
        c0 = tt * TT
        tsl = slice(c0, c0 + TT)
        ops_ = [dma(cx, "sp", xT[:, kc, :], W["x_in"][kc * 128:(kc + 1) * 128, tsl], writes=[xT_b[kc]], dkey="xT")
                for kc in range(NKC)]
        for kc in range(NKC):
            xT_b[kc].w = ops_[-1:]
        if pre:
            ops_ = [dma(cx, "sp", oTs[:, kc, :], W["oT"][kc * 128:(kc + 1) * 128, tsl],
                        writes=[oTs_b] if kc == 0 else [], dkey="oTs") for kc in range(NKC)]
            oTs_b.w = ops_[-1:]
            for m in range(NKC):
                w_t, w_b, wk = w8_r.next()
                dma(cx, "sp", w_t[:], W["wout"][m], writes=[w_b], dkey=wk)
                for h in range(NH):
                    cs = slice(h * 512, (h + 1) * 512)
                    bk, bb = cx.bank()
                    for kc in range(NKC):
                        s.add("pe", I("matmul", bk[:], lhsT=w_t[:, kc, :], rhs=oTs[:, kc, cs],
                                                                              start=(kc == 0), stop=(kc == NKC - 1)),
                              reads=[w_b, oTs_b], writes=[bb], acc=(kc != 0))
                    s.add("dve", I("tensor_tensor", out=xT[:, m, cs], in0=bk[:], in1=xT[:, m, cs],
                                                                       op=ALU.add),
                          reads=[bb], writes=[xT_b[m]] if h == 0 else [], deps=(xT_b[m].w if h else ()))
            ffn(W["wg2"], W["wu2"], W["wd2"], gains["g_ffn2"])
        if post:
            ffn(W["wg1"], W["wu1"], W["wd1"], gains["g_ffn1"])
            for kc in range(NKC):
                cx.outs.append(dma(cx, "pool", W["x_out"][kc * 128:(kc + 1) * 128, tsl], xT[:, kc, :],
                                   reads=[xT_b[kc]], dkey="x1st"))
            for i, nm in enumerate(("cos64", "sin64", "cos32", "sin32")):
                dma(cx, "sp", ropet[i][:], W[nm][:, tsl], writes=[rope_b] if i == 0 else [],
                    deps=() if i == 0 else (), dkey="rope")
                if i:
                    rope_b.w.append(s.q["sp"][-1])
            rope_b.w = rope_b.w[-1:]
            rmsnorm(xT, xT_b, NKC, gains["g_mix"], xn, xn_b, D)

            def proj(name):
                w_t, w_b, wk = w8_r.next()
                dma(cx, "sp", w_t[:], W["win_fm"][WIN_IDX[name]], writes=[w_b], dkey=wk)
                res = []
                for h in range(NH):
                    cs = slice(h * 512, (h + 1) * 512)
                    bk, bb = cx.bank()
                    for kc in range(NKC):
                        s.add("pe", I("matmul",
                            bk[:], lhsT=w_t[:, kc, :], rhs=xn[:, kc, cs], start=(kc == 0), stop=(kc == NKC - 1)),
                              reads=[w_b, xn_b[kc]], writes=[bb], acc=(kc != 0))
                    res.append((bk, bb, cs))
                return res

            def store_fm(stg, stb, dst_ap, prt=slice(0, 128)):
                op = dma(cx, "pool", dst_ap, stg[prt, :], reads=[stb], dkey=None or f"st_{id(stb)}")
                cx.outs.append(op)
                return op

            for nm, dstn, scale in (("a_q", "aqT", 0.125), ("a_k", "akT", 1.0)):
                for c in range(3):
                    stg, stb, _ = st_r.next()
                    for i, (bk, bb, cs) in enumerate(proj(f"{nm}{c}")):
                        s.add("act", I("activation",
                            out=stg[:, cs], in_=bk[:], func=AF.Copy, scale=scale),
                              reads=[bb], writes=[stb] if i == 0 else [], deps=(stb.w if i else ()))
                    store_fm(stg, stb, W[dstn][c, :, tsl])
            for nm, dstn, nchk, ci, si in (("b_q", "bqT", 3, 0, 1), ("b_k", "bkT", 3, 0, 1), ("i_q", "iqT", 3, 2, 3)):
                for c in range(nchk):
                    stg, stb, _ = st_r.next()
                    pa = proj(f"{nm}{c}")
                    pb = proj(f"{nm}{c}s")
                    for i in range(NH):
                        rope_evac(pa[i][0], pa[i][1], pb[i][0], pb[i][1], ci, si, stg,
                                  dict(writes=[stb]) if i == 0 else dict(deps=stb.w), slice(0, 128), pa[i][2])
                    if NH > 1:
                        stb.w = [s.q["pool"][-1]]
                    store_fm(stg, stb, W[dstn][c, :, tsl])
            stg, stb, _ = st_r.next()
            pa = proj("i_k")
            pb = proj("i_ks")
            for i in range(NH):
                rope_evac(pa[i][0], pa[i][1], pb[i][0], pb[i][1], 2, 3, stg,
                          dict(writes=[stb]) if i == 0 else dict(deps=stb.w), slice(0, 128), pa[i][2])
            if NH > 1:
                stb.w = [s.q["pool"][-1]]
            store_fm(stg, stb, W["ikT4"][:, tsl])
            stg, stb, _ = st_r.next()
            pa = proj("c_kr")
            pb = proj("c_krs")
            for i in range(NH):
                rope_evac(pa[i][0], pa[i][1], pb[i][0], pb[i][1], 2, 3, stg,
                          dict(writes=[stb]) if i == 0 else dict(deps=stb.w), slice(64, 96), pa[i][2])
            if NH > 1:
                stb.w = [s.q["pool"][-1]]
            for hh in range(5):
                store_fm(stg, stb, W["ckT"][hh, 64:96, tsl], prt=slice(64, 96))
            for nm, nchk, dst32, dst32_b in (("c_q", 3, cq32, cq32_b), ("c_kv", 2, ckv32, ckv32_b)):
                for c in range(nchk):
                    for i, (bk, bb, cs) in enumerate(proj(f"{nm}{c}")):
                        s.add("act", I("copy", out=dst32[:, c, cs], in_=bk[:]),
                              reads=[bb], writes=[dst32_b[c]] if i == 0 else [], deps=(dst32_b[c].w if i else ()))
            for ts in range(NS):
                tok = slice(ts * 128, (ts + 1) * 128)
                b1, b1b = cx.bank()
                b2, b2b = cx.bank()
                for kc in range(NKC):
                    s.add("pe", I("matmul", b1[:, 0:384], lhsT=xn[:, kc, tok], rhs=wtm[:, kc, 0:384],
                                                                          start=(kc == 0), stop=(kc == NKC - 1)),
                          reads=[wtm_b, xn_b[kc]], writes=[b1b], acc=(kc != 0))
                for kc in range(NKC):
                    s.add("pe", I("matmul", b2[:, 0:328], lhsT=xn[:, kc, tok], rhs=wtm[:, kc, 384:712],
                                                                          start=(kc == 0), stop=(kc == NKC - 1)),
                          reads=[wtm_b, xn_b[kc]], writes=[b2b], acc=(kc != 0))
                a_t, a_b, ak = av_r.next()
                s.add("act", I("copy", out=a_t[:, :, 0:64],
                                                              in_=b1[:, 0:384].rearrange("p (h d) -> p h d", d=64)),
                      reads=[b1b], writes=[a_b])
                cx.outs.append(dma(cx, "pool", W["av"][c0 + ts * 128:c0 + (ts + 1) * 128, :],
                                   a_t[:].rearrange("p h d -> p (h d)"), reads=[a_b], dkey="st" + ak))
                v_t, v_b, vk = bv_r.next()
                s.add("dve", I("tensor_copy", out=v_t[:, :, 0:64],
                                                                     in_=b2[:, 0:320].rearrange("p (h d) -> p h d", d=64)),
                      reads=[b2b], writes=[v_b])
                cx.outs.append(dma(cx, "pool", W["bv"][c0 + ts * 128:c0 + (ts + 1) * 128, :],
                                   v_t[:].rearrange("p h d -> p (h d)"), reads=[v_b], dkey="st" + vk))
                w_t_, w_b_, wk_ = iw_r.next()
                s.add("dve", I("tensor_copy", out=w_t_[:], in_=b2[:, 320:328]),
                      reads=[b2b], writes=[w_b_])
                cx.outs.append(dma(cx, "pool", W["iw"][c0 + ts * 128:c0 + (ts + 1) * 128, :], w_t_[:],
                                   reads=[w_b_], dkey="st" + wk_))
            rmsnorm(cq32, cq32_b, 3, gains["g_cq"], cqn, cqn_b, 384)
            for hh in range(5):
                stg, stb, _ = st_r.next()
                for h in range(NH):
                    cs = slice(h * 512, (h + 1) * 512)
                    bp, bpb = cx.bank()
                    bs_, bsb = cx.bank()
                    for c in range(3):
                        s.add("pe", I("matmul", bp[0:96, :], lhsT=wuq[:, hh, c, :], rhs=cqn[:, c, cs],
                                                                                 start=(c == 0), stop=(c == 2)),
                              reads=[wuq_b, cqn_b[c]], writes=[bpb], acc=(c != 0))
                    for c in range(3):
                        s.add("pe", I("matmul", bs_[0:96, :], lhsT=wuq[:, 5 + hh, c, :], rhs=cqn[:, c, cs],
                                                                                   start=(c == 0), stop=(c == 2)),
                              reads=[wuq_b, cqn_b[c]], writes=[bsb], acc=(c != 0))
                    o1 = s.add("act", I("copy", out=stg[0:64, cs], in_=bp[0:64, :]),
                               reads=[bpb], writes=[stb] if h == 0 else [], deps=(stb.w if h else ()))
                    o2 = rope_evac(bp, bpb, bs_, bsb, 2, 3, stg, dict(deps=[o1]), slice(64, 96), cs)
                    stb.w = [o1, o2]
                cx.outs.append(dma(cx, "pool", W["cqT"][hh, :, tsl], stg[0:96, :], reads=[stb], dkey=f"st_{id(stb)}"))
            rmsnorm(ckv32, ckv32_b, 2, gains["g_ckv"], ckvn, ckvn_b, 256)
            for hh in range(5):
                stg, stb, _ = st_r.next()
                for h in range(NH):
                    cs = slice(h * 512, (h + 1) * 512)
                    bp, bpb = cx.bank()
                    for c in range(2):
                        s.add("pe", I("matmul", bp[0:64, :], lhsT=wkk[:, hh, c, :], rhs=ckvn[:, c, cs],
                                                                                 start=(c == 0), stop=(c == 1)),
                              reads=[wkk_b, ckvn_b[c]], writes=[bpb], acc=(c != 0))
                    s.add("act", I("copy", out=stg[0:64, cs], in_=bp[0:64, :]),
                          reads=[bpb], writes=[stb] if h == 0 else [], deps=(stb.w if h else ()))
                cx.outs.append(dma(cx, "pool", W["ckT"][hh, 0:64, tsl], stg[0:64, :], reads=[stb], dkey=f"st_{id(stb)}"))
            for ts in range(NS):
                tok = slice(ts * 128, (ts + 1) * 128)
                b1, b1b = cx.bank()
                for c in range(2):
                    s.add("pe", I("matmul", b1[:, 0:320], lhsT=ckvn[:, c, tok], rhs=wkv[:, c, :],
                                                                        start=(c == 0), stop=(c == 1)),
                          reads=[wkv_b, ckvn_b[c]], writes=[b1b], acc=(c != 0))
                v_t, v_b, vk = cv_r.next()
                s.add("act", I("copy", out=v_t[:, :, 0:64],
                                                              in_=b1[:, 0:320].rearrange("p (h d) -> p h d", d=64)),
                      reads=[b1b], writes=[v_b])
                cx.outs.append(dma(cx, "pool", W["cv"][c0 + ts * 128:c0 + (ts + 1) * 128, :],
                                   v_t[:].rearrange("p h d -> p (h d)"), reads=[v_b], dkey="st" + vk))
        if final:
            rmsnorm(xT, xT_b, NKC, gains["g_final"], fo, fo_b, D)
            for kc in range(NKC):
                cx.outs.append(dma(cx, "pool", W["y_out"][kc * 128:(kc + 1) * 128, tsl], fo[:, kc, :],
                                   reads=[fo_b[kc]], dkey="yst"))


QK_OUT = dict(
    aqT=lambda T: ([3, 128, T], BF16), akT=lambda T: ([3, 128, T], BF16),
    bqT=lambda T: ([3, 128, T], BF16), bkT=lambda T: ([3, 128, T], BF16),
    iqT=lambda T: ([3, 128, T], BF16), ikT4=lambda T: ([128, T], BF16),
    cqT=lambda T: ([5, 96, T], BF16), ckT=lambda T: ([5, 96, T], BF16),
    av=lambda T: ([T, 390], BF16), bv=lambda T: ([T, 325], BF16), cv=lambda T: ([T, 325], BF16),
    iw=lambda T: ([T, 8], F32),
)
FFN_SH = dict(wg=[NFC, 128, NKC * 128], wu=[NFC, 128, NKC * 128], wd=[NKC, 128, NFC * 128])
POST_SH = dict(win_fm=[NWC, 128, NKC * 128], win_tm=[1, 128, NKC * NTM], wuq=[1, 128, 10 * 3 * 96],
               wukv_k=[1, 128, 5 * 2 * 64], wukv_v=[1, 128, 2 * 320])


def declare_T_weights(cx, pre, post, final, sfx=""):
    W = {}
    todo = []
    if pre:
        for k, sh in FFN_SH.items():
            todo.append((k + "2", sh))
        todo.append(("wout", [NKC, 128, NKC * 128]))
        W["g_ffn2"] = cx.dram("g_ffn2" + sfx, [128, 8], F32, "ExternalInput")
    if post:
        for k, sh in FFN_SH.items():
            todo.append((k + "1", sh))
        for k, sh in POST_SH.items():
            todo.append((k, sh))
        for g, n in (("g_ffn1", 8), ("g_mix", 8), ("g_cq", 3), ("g_ckv", 2)):
            W[g] = cx.dram(g + sfx, [128, n], F32, "ExternalInput")
    if final:
        W["g_final"] = cx.dram("g_final" + sfx, [128, 8], F32, "ExternalInput")
    cst = Caster(cx, 1024)
    for k, sh in todo:
        src = cx.dram(k + sfx + "_f32", sh, F32, "ExternalInput")
        dst = cx.dram(k + sfx + "_bf", sh, BF16, "Internal")
        n, _, f = sh
        for i in range(n):
            step = 1024
            for f0 in range(0, f, step):
                f1 = min(f, f0 + step)
                cst.cast(src[i, :, f0:f1], dst[i, :, f0:f1], f1 - f0)
        W[k] = dst
    for k in ("wg1", "wu1", "wd1", "wg2", "wu2", "wd2"):
        if k in W:
            W[k] = W[k].rearrange("n p (kc m) -> n p kc m", m=128)
    if "wout" in W:
        W["wout"] = W["wout"].rearrange("n p (kc m) -> n p kc m", m=128)
    if post:
        W["win_fm"] = W["win_fm"].rearrange("n p (kc m) -> n p kc m", m=128)
        W["win_tm"] = W["win_tm"][0].rearrange("p (kc m) -> p kc m", m=NTM)
        W["wuq"] = W["wuq"][0].rearrange("p (a c m) -> p a c m", a=10, c=3)
        W["wukv_k"] = W["wukv_k"][0].rearrange("p (a c m) -> p a c m", a=5, c=2)
        W["wukv_v"] = W["wukv_v"][0].rearrange("p (c m) -> p c m", c=2)
    W["_cast_ops"] = cst.last
    return W


def host_T_weights(inp, l_pre, l_post, final, sfx=""):
    d = {}
    f = np.float32
    g8 = lambda v: np.ascontiguousarray(np.asarray(v, f).reshape(-1, 128).T)
    if l_pre is not None:
        l = l_pre
        d["wg2" + sfx + "_f32"] = chunked(inp["ffn2_w_gate"][l]).reshape(FFN_SH["wg"])
        d["wu2" + sfx + "_f32"] = chunked(inp["ffn2_w_up"][l]).reshape(FFN_SH["wu"])
        d["wd2" + sfx + "_f32"] = chunked(inp["ffn2_w_down"][l]).reshape(FFN_SH["wd"])
        d["wout" + sfx + "_f32"] = chunked(inp["w_out"][l]).reshape([NKC, 128, NKC * 128])
        d["g_ffn2" + sfx] = g8(inp["ffn2_norm"][l])
    if l_post is not None:
        l = l_post
        d["wg1" + sfx + "_f32"] = chunked(inp["ffn1_w_gate"][l]).reshape(FFN_SH["wg"])
        d["wu1" + sfx + "_f32"] = chunked(inp["ffn1_w_up"][l]).reshape(FFN_SH["wu"])
        d["wd1" + sfx + "_f32"] = chunked(inp["ffn1_w_down"][l]).reshape(FFN_SH["wd"])
        win = np.asarray(inp["w_in"][l], f)
        cols = np.concatenate([np.asarray(c) for _, c in WIN_CH])
        d["win_fm" + sfx + "_f32"] = chunked(win[:, cols]).reshape(POST_SH["win_fm"])
        d["win_tm" + sfx + "_f32"] = rowfmt(win[:, WIN_TM_COLS]).reshape(POST_SH["win_tm"])
        wuq = np.asarray(inp["c_w_uq"][l], f)
        ucols = np.asarray(uq_columns())
        t = wuq[:, ucols.reshape(-1)].reshape(3, 128, 10, 96).transpose(1, 2, 0, 3)
        d["wuq" + sfx + "_f32"] = np.ascontiguousarray(t).reshape(POST_SH["wuq"])
        wukv = np.asarray(inp["c_w_ukv"][l], f).reshape(2, 128, 5, 128)
        d["wukv_k" + sfx + "_f32"] = np.ascontiguousarray(wukv[:, :, :, 0:64].transpose(1, 2, 0, 3)).reshape(POST_SH["wukv_k"])
        d["wukv_v" + sfx + "_f32"] = np.ascontiguousarray(wukv[:, :, :, 64:128].transpose(1, 0, 2, 3)).reshape(POST_SH["wukv_v"])
        d["g_ffn1" + sfx] = g8(inp["ffn1_norm"][l])
        d["g_mix" + sfx] = g8(inp["mix_norm"][l])
        d["g_cq" + sfx] = g8(inp["c_q_norm"][l])
        d["g_ckv" + sfx] = g8(inp["c_kv_norm"][l])
    if final:
        d["g_final" + sfx] = g8(inp["final_norm"])
    return d


def build_T(Town, TT, pre, post, final):
    cx = Ctx()
    W = declare_T_weights(cx, pre, post, final)
    W["x_in"] = cx.dram("x_in", [D, Town], F32, "ExternalInput")
    if pre:
        W["oT"] = cx.dram("oT", [D, Town], BF16, "ExternalInput")
    if post:
        W["x_out"] = cx.dram("x_out", [D, Town], F32, "ExternalOutput")
        for nm in ("cos64", "sin64", "cos32", "sin32"):
            W[nm] = cx.dram(nm, [128, Town], F32, "ExternalInput")
        for nm, fsh in QK_OUT.items():
            sh, dt = fsh(Town)
            W[nm] = cx.dram(nm, sh, dt, "ExternalOutput")
    if final:
        W["y_out"] = cx.dram("y_out", [D, Town], F32, "ExternalOutput")
    barrier(cx, W.pop("_cast_ops"))
    emit_T(cx, Town, TT, pre, post, final, W)
    return cx.finish()


NBIS = 20
TOPK = 256
A_MS = ((0, 1, 2, 3, 4, 5), (2, 3, 4, 5, 6, 7))
QOFF = ((0, 3), (1, 2))


def host_M_consts(r, rel_bias):
    d = {}
    kl = np.arange(128)[:, None]
    ql = np.arange(128)[None, :]
    ab = np.zeros((128, 2, 6, 6, 128), np.float32)
    am = np.zeros((128, 2, 6, 128), np.float32)
    for sl in range(2):
        for mi, m in enumerate(A_MS[sl]):
            delta = QOFF[r][sl] + 4 - m
            rel = 128 * delta + ql - kl
            qc = 2 * (QOFF[r][sl] + 4) + ql // 64
            kc = 2 * m + kl // 64
            valid = (qc - kc >= 0) & (qc - kc <= 8)
            ridx = np.clip(rel, -63, 128) + 63
            for h in range(6):
                ab[:, sl, h, mi, :] = rel_bias[h][ridx]
            am[:, sl, mi, :] = np.where(valid, 0.0, NEGM)
    d["abias"] = ab
    d["amask"] = am
    cm = np.zeros((128, 2, 4, 128), np.float32)
    im = np.zeros((128, 2, 4, 128), np.float32)
    for sl in range(2):
        for kt in range(4):
            qc = 2 * QOFF[r][sl] + ql // 64
            kc = 2 * kt + kl // 64
            cm[:, sl, kt, :] = np.where(kc <= qc, 1.0, 0.0)
            im[:, sl, kt, :] = np.where(kc <= qc, 0.0, -1e30).T
    d["cmask"] = cm.astype(ml_dtypes.bfloat16)
    d["imask"] = im.reshape(128, 2, 512)
    d["ident"] = np.eye(128, dtype=np.float32).astype(ml_dtypes.bfloat16)
    return d


def emit_M(cx, NG, W):
    s = cx.s
    RB = (0, 1, 2, 3, 4)
    OBK = (5, 6, 7)
    T = NG * 512

    ident = cx.sb([128, 128], BF16, "ident")
    ident_b = Buf()
    dma(cx, "sp", ident[:], W["ident"], writes=[ident_b], dkey="ident")
    cmask = cx.sb([128, 2, 512], BF16, "cmask")
    cmask_b = Buf()
    dma(cx, "sp", cmask[:], W["cmask"].rearrange("p s k q -> p s (k q)"), writes=[cmask_b], dkey="cmask")
    imask = cx.sb([128, 2, 512], F32, "imask")
    imask_b = Buf()
    dma(cx, "sp", imask[:], W["imask"], writes=[imask_b], dkey="imask")
    abm = cx.sb([128, 2, 6, 6, 128], BF16, "abm")
    abm_b = Buf()
    amk = cx.sb([128, 2, 6, 128], F32, "amk")
    amk_b = Buf()
    dma(cx, "sp", amk[:], W["amask"], writes=[amk_b], dkey="amk")
    abr = Ring(cx, 2, [128, 6, 128], F32, "abr")
    ops = []
    for sl in range(2):
        for h in range(6):
            t, b, k = abr.next()
            dma(cx, "sp", t[:], W["abias"][:, sl, h], writes=[b], dkey=k)
            ops.append(s.add("dve", I("tensor_tensor", out=abm[:, sl, h], in0=t[:], in1=amk[:, sl],
                                                                                 op=ALU.add), reads=[b, amk_b]))
    abm_b.w = ops[-1:]

    scores = cx.sb([128, T], F32, "scores")
    sc_b = [Buf() for _ in range(NG)]
    nmask = cx.sb([128, 2, T], BF16, "nmask")
    nm_b = [Buf(), Buf()]
    ik_r = Ring(cx, 3, [128, 512], BF16, "ik")
    bk_r = Ring(cx, 2, [128, 3, 512], BF16, "bk")
    ck_r = Ring(cx, 2, [96, 5, 512], BF16, "ck")
    bv_r = Ring(cx, 2, [128, 4, 325], BF16, "bvk")
    cv_r = Ring(cx, 2, [128, 4, 325], BF16, "cvk")
    aq_r = Ring(cx, 2, [128, 3, 256], BF16, "aq")
    bq_r = Ring(cx, 2, [128, 3, 256], BF16, "bq")
    iq_r = Ring(cx, 2, [128, 3, 256], BF16, "iq")
    cq_r = Ring(cx, 2, [96, 5, 256], BF16, "cq")
    iw_r = Ring(cx, 2, [128, 2, 8], F32, "iwq")
    ak_r = Ring(cx, 1, [128, 3, 1024], BF16, "ak")
    av_r = Ring(cx, 1, [128, 8, 390], BF16, "avk")
    pt_r = Ring(cx, 3, [128, 768], BF16, "pt")
    r_r = Ring(cx, 3, [128, 512], F32, "rr")
    otok = cx.sb([128, 2, D], BF16, "otok")
    otok_b = [Buf(), Buf()]
    oTs_r = Ring(cx, 2, [128, NKC, 256], BF16, "oTs")
    stat = cx.sb([128, 2, NG], F32, "stat")
    stat_b = Buf()
    sm = {nm: cx.sb([128, 1], F32, "bs_" + nm) for nm in ("lo", "hi", "wd", "mid", "cnt", "t", "t2")}
    sm_b = {nm: Buf() for nm in sm}
    rl_r = Ring(cx, 2, [128, 12], F32, "rl")
    fresh = {}

    def acc_loc(aid):
        bi = OBK[aid // 7]
        return bi, (aid % 7) * 65

    def pv(aid, lhsT, rhs, reads, last):
        bi, off = acc_loc(aid)
        st = fresh.get(bi, True)
        fresh[bi] = False
        return s.add("pe", I("matmul", cx.banks[bi][:, off:off + 65], lhsT=lhsT, rhs=rhs, start=st, stop=last,
                                              skip_group_check=True),
                     reads=reads, writes=[cx.bankb[bi]], acc=not st)

    def normalize(aids, sl, col0, hd):
        rl, rlb, _ = rl_r.next()
        n = len(aids)
        for k, aid in enumerate(aids):
            bi, off = acc_loc(aid)
            s.add("dve", I("reciprocal", out=rl[:, k:k + 1], in_=cx.banks[bi][:, off + 64:off + 65]),
                  reads=[cx.bankb[bi]], writes=[rlb] if k == 0 else [], deps=rlb.w if k else ())
            rlb.w = [s.q["dve"][-1]]
        for k, aid in enumerate(aids):
            bi, off = acc_loc(aid)
            o = s.add("dve", I("tensor_scalar",
                out=otok[:, sl, col0 + k * hd:col0 + (k + 1) * hd], in0=cx.banks[bi][:, off:off + hd],
                scalar1=rl[:, k:k + 1], scalar2=None, op0=ALU.mult),
                      reads=[cx.bankb[bi], rlb], deps=otok_b[sl].r if k == 0 else ())
            otok_b[sl].w.append(o)

    import os
    if os.environ.get('DBG_BAR'):
        barrier(cx)
    for i in ([0] if os.environ.get('DBG_REP0') else []) + list(range(NG)):
        q0 = i * 256
        aq, aq_b, aqk = aq_r.next()
        bq, bq_b, bqk = bq_r.next()
        iq, iq_b, iqk = iq_r.next()
        cq, cq_b, cqk = cq_r.next()
        iwq, iwq_b, iwk = iw_r.next()
        def ld_c(dst, dst_b, key, srcT, nchunk, c0_, c1_, dcol=0):
            ops_ = []
            for c in range(nchunk):
                ops_.append(dma(cx, "sp", dst[:, c, dcol:dcol + (c1_ - c0_)], srcT[c, :, c0_:c1_],
                                writes=[dst_b] if c == 0 else [], dkey=key))
            dst_b.w = ops_[-1:]

        def ld_tok(dst, dst_b, key, srcT, t0_, ntile, dt0=0):
            ops_ = []
            for k_ in range(ntile):
                ops_.append(dma(cx, "sp", dst[:, dt0 + k_, :], srcT[t0_ + k_ * 128:t0_ + (k_ + 1) * 128, :],
                                writes=[dst_b] if k_ == 0 else [], dkey=key))
            dst_b.w = ops_[-1:]

        ld_c(aq, aq_b, aqk, W["aqT"], 3, q0, q0 + 256)
        ld_c(bq, bq_b, bqk, W["bqT"], 3, q0, q0 + 256)
        ld_c(iq, iq_b, iqk, W["iqT"], 3, q0, q0 + 256)
        ld_c(cq, cq_b, cqk, W["cqT"], 5, q0, q0 + 256)
        ld_tok(iwq, iwq_b, iwk, W["iw"], q0, 2)
        for sl in range(2):
            otok_b[sl].w = []
        if "dbg_cq" in W and i == 0:
            cx.outs.append(dma(cx, "pool", W["dbg_cq"], cq[:], reads=[cq_b], dkey="dbgcq"))
            cx.outs.append(dma(cx, "pool", W["dbg_bq"], bq[:], reads=[bq_b], dkey="dbgbq"))
        ak, ak_b, akk = ak_r.next()
        avk, avk_b, avkk = av_r.next()
        k0 = (i - 1) * 512
        if i == 0:
            ld_c(ak, ak_b, akk, W["akT"], 3, 0, 512, dcol=512)
            ld_tok(avk, avk_b, avkk, W["av"], 0, 4, dt0=4)
        else:
            ld_c(ak, ak_b, akk, W["akT"], 3, k0, k0 + 1024)
            ld_tok(avk, avk_b, avkk, W["av"], k0, 8)
        for b_ in OBK:
            fresh[b_] = True
        for sl in range(2):
            qs = slice(sl * 128, (sl + 1) * 128)
            for h in range(6):
                pr, hp = divmod(h, 2)
                ps = slice(hp * 64, hp * 64 + 64)
                ms = [(mi, m) for mi, m in enumerate(A_MS[sl]) if (i > 0 or m >= 4)]
                b0, b0b = cx.bank(RB)
                b1, b1b = cx.bank(RB)
                first = {0: True, 1: True}
                for mi, m in ms:
                    bk, bb = (b0, b0b) if mi < 4 else (b1, b1b)
                    col = (mi % 4) * 128
                    s.add("pe", I("matmul",
                        bk[:, col:col + 128], lhsT=ak[ps, pr, m * 128:(m + 1) * 128], rhs=aq[ps, pr, qs],
                        start=True, stop=False),
                          reads=[ak_b, aq_b], writes=[bb], acc=not first[mi // 4])
                    first[mi // 4] = False
                    s.add("pe", I("matmul",
                        bk[:, col:col + 128], lhsT=ident[:], rhs=abm[:, sl, h, mi, :], start=False, stop=True),
                          reads=[ident_b, abm_b], writes=[bb], acc=True)
                pt, ptb, _ = pt_r.next()
                lo_m = [mi for mi, m in ms if mi < 4]
                hi_m = [mi for mi, m in ms if mi >= 4]
                wops = []
                if lo_m:
                    c_a, c_b = min(lo_m) * 128, (max(lo_m) + 1) * 128
                    wops.append(s.add("act", I("activation",
                        out=pt[:, c_a:c_b], in_=b0[:, c_a:c_b], func=AF.Exp), reads=[b0b], writes=[ptb]))
                c_a, c_b = (min(hi_m) - 4) * 128, (max(hi_m) - 3) * 128
                wops.append(s.add("act", I("activation",
                    out=pt[:, 512 + c_a:512 + c_b], in_=b1[:, c_a:c_b], func=AF.Exp), reads=[b1b],
                    writes=[] if lo_m else [ptb], deps=ptb.w if lo_m else ()))
                ptb.w = wops
                for n_, (mi, m) in enumerate(ms):
                    pv(sl * 6 + h, pt[:, mi * 128:(mi + 1) * 128], avk[:, m, h * 65:(h + 1) * 65],
                       [ptb, avk_b], n_ == len(ms) - 1)
        for sl in range(2):
            normalize([sl * 6 + h for h in range(6)], sl, 0, 64)
        for b_ in OBK:
            fresh[b_] = True

        for sl in range(2):
            qs = slice(sl * 128, (sl + 1) * 128)
            for ks in range(i + 1):
                ikt, ikb, ikk = ik_r.next()
                dma(cx, "sp", ikt[:], W["ikT4"][:, ks * 512:(ks + 1) * 512], writes=[ikb], dkey=ikk)
                blk = scores[:, ks * 512:(ks + 1) * 512]
                for hd in range(8):
                    g, hh = divmod(hd, 3)
                    ps = slice(32 * hh, 32 * hh + 32)
                    bk, bb = cx.bank(RB)
                    s.add("pe", I("matmul",
                        bk[:], lhsT=iq[ps, g, qs], rhs=ikt[ps, :], start=True, stop=True),
                          reads=[iq_b, ikb], writes=[bb])
                    rr, rrb, _ = r_r.next()
                    s.add("act", I("activation", out=rr[:], in_=bk[:], func=AF.Relu),
                          reads=[bb], writes=[rrb])
                    if hd == 0:
                        s.add("dve", I("tensor_scalar",
                            out=blk, in0=rr[:], scalar1=iwq[:, sl, hd:hd + 1], scalar2=None, op0=ALU.mult),
                              reads=[rrb, iwq_b], writes=[sc_b[ks]])
                    else:
                        s.add("dve", I("scalar_tensor_tensor",
                            out=blk, in0=rr[:], scalar=iwq[:, sl, hd:hd + 1], in1=blk, op0=ALU.mult, op1=ALU.add),
                              reads=[rrb, iwq_b], writes=[sc_b[ks]])
                o1 = s.add("dve", I("tensor_reduce", out=stat[:, 0, ks:ks + 1], in_=blk, axis=AX.X,
                                                                            op=ALU.min),
                           reads=[sc_b[ks]], deps=(stat_b.r + stat_b.w) if ks == 0 else ())
                if ks == i:
                    s.add("dve", I("tensor_tensor", out=blk, in0=blk, in1=imask[:, sl, :], op=ALU.add),
                          reads=[imask_b], writes=[sc_b[ks]])
                o2 = s.add("dve", I("tensor_reduce", out=stat[:, 1, ks:ks + 1], in_=blk, axis=AX.X,
                                                                            op=ALU.max),
                           reads=[sc_b[ks]])
                if ks == 0:
                    stat_b.w = []
                    stat_b.r = []
                stat_b.w = [o2]
            n = (i + 1) * 512
            s.add("dve", I("tensor_reduce", out=sm["lo"][:], in_=stat[:, 0, 0:i + 1], axis=AX.X, op=ALU.min),
                  reads=[stat_b], writes=[sm_b["lo"]])
            s.add("dve", I("tensor_reduce", out=sm["hi"][:], in_=stat[:, 1, 0:i + 1], axis=AX.X, op=ALU.max),
                  reads=[stat_b], writes=[sm_b["hi"]])
            s.add("dve", I("tensor_tensor", out=sm["wd"][:], in0=sm["hi"][:], in1=sm["lo"][:], op=ALU.subtract),
                  reads=[sm_b["hi"], sm_b["lo"]], writes=[sm_b["wd"]])
            for it in range(NBIS):
                f = 0.5 ** (it + 1)
                s.add("dve", I("scalar_tensor_tensor", out=sm["mid"][:], in0=sm["wd"][:], scalar=f, in1=sm["lo"][:],
                                                                   op0=ALU.mult, op1=ALU.add),
                      reads=[sm_b["wd"], sm_b["lo"]], writes=[sm_b["mid"]])
                s.add("dve", I("tensor_scalar", out=nmask[:, sl, 0:n], in0=scores[:, 0:n], scalar1=sm["mid"][:, 0:1],
                                                            scalar2=None, op0=ALU.is_ge, op1=ALU.add, accum_out=sm["cnt"][:]),
                      reads=[sm_b["mid"]] + sc_b[0:i + 1], writes=[nm_b[sl], sm_b["cnt"]])
                s.add("dve", I("tensor_scalar", out=sm["t"][:], in0=sm["cnt"][:], scalar1=TOPK - 0.5, scalar2=f,
                                                            op0=ALU.is_ge, op1=ALU.mult),
                      reads=[sm_b["cnt"]], writes=[sm_b["t"]])
                s.add("dve", I("scalar_tensor_tensor", out=sm["lo"][:], in0=sm["t"][:], scalar=sm["wd"][:, 0:1],
                                                              in1=sm["lo"][:], op0=ALU.mult, op1=ALU.add),
                      reads=[sm_b["t"], sm_b["wd"]], writes=[sm_b["lo"]])
            s.add("dve", I("tensor_scalar", out=nmask[:, sl, 0:n], in0=scores[:, 0:n],
                                                               scalar1=sm["lo"][:, 0:1], scalar2=NEGM, op0=ALU.is_lt, op1=ALU.mult),
                  reads=[sm_b["lo"]] + sc_b[0:i + 1], writes=[nm_b[sl]])

        for ks in range(i + 1):
            bk_t, bk_b, bkk = bk_r.next()
            ck_t, ck_b, ckk = ck_r.next()
            bv_t, bv_b, bvk = bv_r.next()
            cv_t, cv_b, cvk = cv_r.next()
            ksl = slice(ks * 512, (ks + 1) * 512)
            ld_c(bk_t, bk_b, bkk, W["bkT"], 3, ks * 512, (ks + 1) * 512)
            ld_c(ck_t, ck_b, ckk, W["ckT"], 5, ks * 512, (ks + 1) * 512)
            ld_tok(bv_t, bv_b, bvk, W["bv"], ks * 512, 4)
            ld_tok(cv_t, cv_b, cvk, W["cv"], ks * 512, 4)
            lastk = ks == i
            if "dbg_ck" in W and i == 0:
                cx.outs.append(dma(cx, "pool", W["dbg_ck"], ck_t[:], reads=[ck_b], dkey="dbgck"))
            for sl in range(2):
                qs = slice(sl * 128, (sl + 1) * 128)
                for h in range(5):
                    pr, hp = divmod(h, 2)
                    ps = slice(hp * 64, hp * 64 + 64)
                    bk, bb = cx.bank(RB)
                    for kt in range(4):
                        cs = slice(kt * 128, (kt + 1) * 128)
                        s.add("pe", I("matmul",
                            bk[:, cs], lhsT=bk_t[ps, pr, cs], rhs=bq[ps, pr, qs], start=True, stop=False),
                              reads=[bk_b, bq_b], writes=[bb], acc=(kt != 0))
                        s.add("pe", I("matmul",
                            bk[:, cs], lhsT=nmask[:, sl, ks * 512 + kt * 128:ks * 512 + (kt + 1) * 128], rhs=ident[:],
                            start=False, stop=True),
                              reads=[nm_b[sl], ident_b], writes=[bb], acc=True)
                    pt, ptb, _ = pt_r.next()
                    s.add("act", I("activation", out=pt[:, 0:512], in_=bk[:], func=AF.Exp, scale=0.125),
                          reads=[bb], writes=[ptb])
                    for kt in range(4):
                        pv(sl * 10 + h, pt[:, kt * 128:(kt + 1) * 128], bv_t[:, kt, h * 65:(h + 1) * 65],
                           [ptb, bv_b], lastk and kt == 3)
                    bk, bb = cx.bank(RB)
                    for kt in range(4):
                        cs = slice(kt * 128, (kt + 1) * 128)
                        s.add("pe", I("matmul",
                            bk[:, cs], lhsT=ck_t[:, h, cs], rhs=cq[:, h, qs], start=True, stop=True),
                              reads=[ck_b, cq_b], writes=[bb], acc=(kt != 0))
                    pt, ptb, _ = pt_r.next()
                    if "dbg_st" in W and i == 0 and sl == 0 and h == 0:
                        dt_ = cx.sb([128, 512], F32, "dbgst")
                        dtb = Buf()
                        s.add("dve", I("tensor_copy", out=dt_[:], in_=bk[:]), reads=[bb], writes=[dtb])
                        cx.outs.append(dma(cx, "sp", W["dbg_st"], dt_[:], reads=[dtb], dkey="dbgst"))
                    s.add("act", I("activation", out=pt[:, 0:512], in_=bk[:], func=AF.Exp,
                                                                      scale=float(96 ** -0.5)),
                          reads=[bb], writes=[ptb])
                    if "dbg_st" in W and i == 0 and sl == 0 and h == 0:
                        cx.outs.append(dma(cx, "sp", W["dbg_pt"], pt[:, 0:512], reads=[ptb], dkey="dbgpt"))
                    if lastk:
                        s.add("pool", I("tensor_tensor", out=pt[:, 0:512], in0=pt[:, 0:512],
                                                                              in1=cmask[:, sl, :], op=ALU.mult),
                              reads=[cmask_b], writes=[ptb])
                    for kt in range(4):
                        pv(sl * 10 + 5 + h, pt[:, kt * 128:(kt + 1) * 128], cv_t[:, kt, h * 65:(h + 1) * 65],
                           [ptb, cv_b], lastk and kt == 3)
        for sl in range(2):
            normalize([sl * 10 + h for h in range(5)], sl, 384, 64)
            normalize([sl * 10 + 5 + h for h in range(5)], sl, 704, 64)
        for b_ in OBK:
            fresh[b_] = True

        if "dbg_otok" in W:
            cx.outs.append(dma(cx, "sp", W["dbg_otok"][i], otok[:], reads=otok_b, dkey=f"dbg{i}"))
        oTs, oTs_b, oTk = oTs_r.next()
        wops = []
        for sl in range(2):
            otok_b[sl].r = []
            for half in range(2):
                bk, bb = cx.bank(RB)
                bkv = bk[:].bitcast(BF16)
                for c4 in range(4):
                    kc = half * 4 + c4
                    s.add("pe", I("transpose",
                        bkv[:, c4 * 128:(c4 + 1) * 128], otok[:, sl, kc * 128:(kc + 1) * 128], ident[:]),
                          reads=[otok_b[sl], ident_b], writes=[bb], acc=(c4 != 0))
                wops.append(s.add("act", I("copy",
                    out=oTs[:, half * 4:half * 4 + 4, sl * 128:(sl + 1) * 128],
                    in_=bkv[:, 0:512].rearrange("p (c q) -> p c q", q=128)),
                    reads=[bb], deps=(oTs_b.w + oTs_b.r) if not wops else ()))
        oTs_b.w = wops
        oTs_b.r = []
        for kc in range(NKC):
            cx.outs.append(dma(cx, "pool", W["oT"][kc * 128:(kc + 1) * 128, q0:q0 + 256], oTs[:, kc, :],
                               reads=[oTs_b], dkey="st" + oTk))


def build_M(NG):
    cx = Ctx()
    Town, T = NG * 256, NG * 512
    W = {}
    for nm in ("aqT", "bqT", "iqT", "cqT", "iw"):
        sh, dt = QK_OUT[nm](Town)
        W[nm] = cx.dram(nm, sh, dt, "ExternalInput")
    for nm in ("akT", "bkT", "ikT4", "ckT", "av", "bv", "cv"):
        sh, dt = QK_OUT[nm](T)
        W[nm] = cx.dram(nm, sh, dt, "ExternalInput")
    W["abias"] = cx.dram("abias", [128, 2, 6, 6, 128], F32, "ExternalInput")
    W["amask"] = cx.dram("amask", [128, 2, 6, 128], F32, "ExternalInput")
    W["cmask"] = cx.dram("cmask", [128, 2, 4, 128], BF16, "ExternalInput")
    W["imask"] = cx.dram("imask", [128, 2, 512], F32, "ExternalInput")
    W["ident"] = cx.dram("ident", [128, 128], BF16, "ExternalInput")
    W["oT"] = cx.dram("oT", [D, Town], BF16, "ExternalOutput")
    import os
    if os.environ.get("DBG_OTOK"):
        W["dbg_otok"] = cx.dram("dbg_otok", [NG, 128, 2, D], BF16, "ExternalOutput")
        W["dbg_cq"] = cx.dram("dbg_cq", [96, 5, 256], BF16, "ExternalOutput")
        W["dbg_st"] = cx.dram("dbg_st", [128, 512], F32, "ExternalOutput")
        W["dbg_ck"] = cx.dram("dbg_ck", [96, 5, 512], BF16, "ExternalOutput")
        W["dbg_pt"] = cx.dram("dbg_pt", [128, 512], BF16, "ExternalOutput")
        W["dbg_bq"] = cx.dram("dbg_bq", [128, 3, 256], BF16, "ExternalOutput")
    emit_M(cx, NG, W)
    return cx.finish()


NCORES = 8
SEQ = 8192
TOWN = 4096
TT_T = 512
NG_M = 16
KSIDE = ("akT", "bkT", "ikT4", "ckT", "av", "bv", "cv")
QSIDE = ("aqT", "bqT", "iqT", "cqT", "iw")
_PROG = {}


def _prog(key, fn):
    if key not in _PROG:
        _PROG[key] = fn()
    return _PROG[key]


def _core_tokens(r):
    tiles = [tile_of(r, j) for j in range(SEQ // 256)]
    return np.concatenate([np.arange(t * 128, (t + 1) * 128) for t in tiles])


def _exchange(res, b):
    out = {}
    owner = {}
    for r in range(2):
        for j in range(SEQ // 256):
            owner[tile_of(r, j)] = (r, j)
    for nm in KSIDE:
        parts = []
        tok_major = nm in ("av", "bv", "cv")
        for g in range(SEQ // 128):
            r, j = owner[g]
            a = np.asarray(res[2 * b + r][nm])
            parts.append(a[j * 128:(j + 1) * 128] if tok_major else a[..., j * 128:(j + 1) * 128])
        out[nm] = np.ascontiguousarray(np.concatenate(parts, axis=0 if tok_major else -1))
    return out


def kernel(**inp):
    inp = {k: np.asarray(v) for k, v in inp.items()}
    x = inp["x"]
    B = x.shape[0]
    toks = [_core_tokens(r) for r in range(2)]
    ropes = [rope_tables(toks[r]) for r in range(2)]
    cores = list(range(NCORES))

    def run(nc, maps):
        return run_bass_kernel_spmd(nc, maps, core_ids=cores).results

    nc = _prog("T0", lambda: build_T(TOWN, TT_T, pre=False, post=True, final=False))
    wts = host_T_weights(inp, None, 0, False)
    maps = []
    for c in cores:
        b, r = divmod(c, 2)
        d = dict(wts)
        d["x_in"] = np.ascontiguousarray(x[b][toks[r]].T)
        d.update(cos64=ropes[r][0], sin64=ropes[r][1], cos32=ropes[r][2], sin32=ropes[r][3])
        maps.append(d)
    res = run(nc, maps)
    xcur = [res[c]["x_out"] for c in cores]
    for l in range(2):
        ncm = _prog("M", lambda: build_M(NG_M))
        kglob = [_exchange(res, b) for b in range(B)]
        consts = [host_M_consts(r, np.asarray(inp["a_rel_bias"][l], np.float32)) for r in range(2)]
        maps = []
        for c in cores:
            b, r = divmod(c, 2)
            d = {nm: res[c][nm] for nm in QSIDE}
            d.update(kglob[b])
            d.update(consts[r])
            maps.append(d)
        resm = run(ncm, maps)
        last = l == 1
        if not last:
            nct = _prog("T1", lambda: build_T(TOWN, TT_T, pre=True, post=True, final=False))
            wts = host_T_weights(inp, l, l + 1, False)
        else:
            nct = _prog("T2", lambda: build_T(TOWN, TT_T, pre=True, post=False, final=True))
            wts = host_T_weights(inp, l, None, True)
        maps = []
        for c in cores:
            b, r = divmod(c, 2)
            d = dict(wts)
            d["x_in"] = xcur[c]
            d["oT"] = resm[c]["oT"]
            if not last:
                d.update(cos64=ropes[r][0], sin64=ropes[r][1], cos32=ropes[r][2], sin32=ropes[r][3])
            maps.append(d)
        res = run(nct, maps)
        if not last:
            xcur = [res[c]["x_out"] for c in cores]
    out = np.empty(x.shape, np.float32)
    for c in cores:
        b, r = divmod(c, 2)
        out[b][toks[r]] = np.asarray(res[c]["y_out"]).T
    return out
```
